# Optimizing a Trainium2 kernel written in Bass

```python
import jax
import jax.numpy as jnp
from jax import lax
import numpy as np

D_MODEL = 2048
BATCH = 1
SEQ = 8192
DEPTH = 4

GRID_W = 64
CTX_LEN = 256
MIX_W = D_MODEL
ATTN_W = MIX_W // 2
CONV_W = MIX_W - ATTN_W
HEAD_DIM = 64
N_HEADS = ATTN_W // HEAD_DIM
N_KV_HEADS = 2
KV_GROUP = N_HEADS // N_KV_HEADS
KV_W = N_KV_HEADS * HEAD_DIM
IN_COLS = ATTN_W + 2 * KV_W + 3 * CONV_W
WINDOW = 128
ATTN_BLOCK = 128
CONV_SIZE = 3
ROPE_THETA = 10000.0
ROPE_AXIS_DIM = HEAD_DIM // 2
N_EXPERTS = 16
N_GROUPS = 4
EXPERTS_PER_GROUP = N_EXPERTS // N_GROUPS
TOP_K = 2
D_FF_EXPERT = 1024
MOE_BLOCK = 128
EPS = 1e-6
NEG_INF = -1e30

kernel_name = 'hybrid_dit_conv_swa_grouped_moe'


def rmsnorm(x, g):
    xf = x.astype(jnp.float32)
    y = xf * lax.rsqrt(jnp.mean(xf * xf, axis=-1, keepdims=True) + EPS)
    return (y * g.astype(jnp.float32)).astype(x.dtype)


def modulate(h, shift, scale):
    return h * (1 + scale) + shift


def rope_axis(x, ang):
    cos = jnp.cos(ang).astype(x.dtype)[None, :, None, :]
    sin = jnp.sin(ang).astype(x.dtype)[None, :, None, :]
    x1, x2 = jnp.split(x, 2, axis=-1)
    return jnp.concatenate([x1 * cos - x2 * sin, x2 * cos + x1 * sin], axis=-1)


def rope_2d(x, ang_row, ang_col):
    return jnp.concatenate([rope_axis(x[..., :ROPE_AXIS_DIM], ang_row),
                            rope_axis(x[..., ROPE_AXIS_DIM:], ang_col)], axis=-1)


def dwconv3(u, w):
    L = u.shape[1]
    up = jnp.pad(u, ((0, 0), (1, 1), (0, 0)))
    return w[0] * up[:, :L] + w[1] * up[:, 1:L + 1] + w[2] * up[:, 2:L + 2]


def split_in(p):
    o = ATTN_W + 2 * KV_W
    q = p[..., :ATTN_W]
    k = p[..., ATTN_W:ATTN_W + KV_W]
    v = p[..., ATTN_W + KV_W:o]
    u = p[..., o:o + CONV_W]
    b_gate = p[..., o + CONV_W:o + 2 * CONV_W]
    c_gate = p[..., o + 2 * CONV_W:]
    return q, k, v, u, b_gate, c_gate


def window_attention(q, k, v, k_ctx, v_ctx, sink):
    B, L = q.shape[:2]
    C = k_ctx.shape[1]
    nb = L // ATTN_BLOCK
    nw = 3 * ATTN_BLOCK
    qb = q.reshape(B, nb, ATTN_BLOCK, N_KV_HEADS, KV_GROUP, HEAD_DIM)

    def band(t):
        tb = jnp.pad(t, ((0, 0), (ATTN_BLOCK, ATTN_BLOCK), (0, 0), (0, 0)))
        tb = tb.reshape(B, nb + 2, ATTN_BLOCK, N_KV_HEADS, HEAD_DIM)
        return jnp.concatenate([tb[:, :-2], tb[:, 1:-1], tb[:, 2:]], axis=2)

    k_win, v_win = band(k), band(v)
    scale = HEAD_DIM ** -0.5
    s_win = jnp.einsum('bnqhgd,bnkhd->bhgnqk', qb, k_win).astype(jnp.float32) * scale
    s_ctx = jnp.einsum('bnqhgd,bchd->bhgnqc', qb, k_ctx).astype(jnp.float32) * scale
    qi = jnp.arange(ATTN_BLOCK)[:, None]
    kj = jnp.arange(nw)[None, :]
    in_window = jnp.abs(qi + ATTN_BLOCK - kj) <= WINDOW
    key_pos = jnp.arange(nb)[:, None] * ATTN_BLOCK - ATTN_BLOCK + kj
    in_seq = (key_pos >= 0) & (key_pos < L)
    mask = in_window[None] & in_seq[:, None, :]
    s_win = jnp.where(mask, s_win, NEG_INF)
    s_sink = jnp.broadcast_to(sink.astype(jnp.float32).reshape(1, N_KV_HEADS, KV_GROUP, 1, 1, 1),
                              s_win.shape[:-1] + (1,))
    p = jax.nn.softmax(jnp.concatenate([s_win, s_ctx, s_sink], axis=-1), axis=-1).astype(v.dtype)
    out = (jnp.einsum('bhgnqk,bnkhd->bnqhgd', p[..., :nw], v_win)
           + jnp.einsum('bhgnqc,bchd->bnqhgd', p[..., nw:nw + C], v_ctx))
    return out.reshape(B, L, ATTN_W)


def context_attention(q, k, v, sink):
    B, C = q.shape[:2]
    qg = q.reshape(B, C, N_KV_HEADS, KV_GROUP, HEAD_DIM)
    s = jnp.einsum('bqhgd,bkhd->bhgqk', qg, k).astype(jnp.float32) * (HEAD_DIM ** -0.5)
    s_sink = jnp.broadcast_to(sink.astype(jnp.float32).reshape(1, N_KV_HEADS, KV_GROUP, 1, 1),
                              s.shape[:-1] + (1,))
    p = jax.nn.softmax(jnp.concatenate([s, s_sink], axis=-1), axis=-1).astype(v.dtype)
    out = jnp.einsum('bhgqk,bkhd->bqhgd', p[..., :C], v)
    return out.reshape(B, C, ATTN_W)


def grouped_moe(h, w_router, router_bias, w_gate, w_up, w_down):
    T, D = h.shape
    logits = jnp.dot(h.astype(jnp.float32), w_router.astype(jnp.float32))
    scores = jax.nn.sigmoid(logits)
    sel = (scores + router_bias.astype(jnp.float32)).reshape(T, N_GROUPS, EXPERTS_PER_GROUP)
    group_score = lax.top_k(sel, TOP_K)[0].sum(-1)
    g_idx = jnp.argmax(group_score, axis=-1).astype(jnp.int32)
    in_group = jnp.take_along_axis(sel, g_idx[:, None, None], axis=1)[:, 0]
    _, local = lax.top_k(in_group, TOP_K)
    expert_idx = g_idx[:, None] * EXPERTS_PER_GROUP + local.astype(jnp.int32)
    sel_scores = jnp.take_along_axis(scores, expert_idx, axis=1)
    gates = sel_scores / jnp.sum(sel_scores, axis=-1, keepdims=True)

    A = T * TOP_K
    flat_e = expert_idx.reshape(A)
    flat_tok = jnp.arange(A, dtype=jnp.int32) // TOP_K
    order = jnp.argsort(flat_e)
    sorted_e = flat_e[order]
    counts = jnp.bincount(flat_e, length=N_EXPERTS).astype(jnp.int32)
    padded = (counts + MOE_BLOCK - 1) // MOE_BLOCK * MOE_BLOCK
    start = jnp.cumsum(counts) - counts
    pend = jnp.cumsum(padded)
    pstart = pend - padded
    dest_sorted = pstart[sorted_e] + (jnp.arange(A, dtype=jnp.int32) - start[sorted_e])
    nblk = -(-(A + N_EXPERTS * (MOE_BLOCK - 1)) // MOE_BLOCK)
    row_tok = jnp.full((nblk * MOE_BLOCK,), T, dtype=jnp.int32).at[dest_sorted].set(flat_tok[order])
    block_expert = jnp.clip(jnp.searchsorted(pend, jnp.arange(nblk, dtype=jnp.int32) * MOE_BLOCK,
                                             side='right'), 0, N_EXPERTS - 1).astype(jnp.int32)
    h_pad = jnp.concatenate([h, jnp.zeros((1, D), h.dtype)], axis=0)

    def expert_block(args):
        tok, e = args
        xb = h_pad[tok]
        a = jnp.dot(xb, w_gate[e])
        b = jnp.dot(xb, w_up[e])
        return jnp.dot(jax.nn.silu(a) * b, w_down[e])

    y_rows = lax.map(expert_block, (row_tok.reshape(nblk, MOE_BLOCK), block_expert))
    y_rows = y_rows.reshape(nblk * MOE_BLOCK, D)
    dest = jnp.zeros((A,), jnp.int32).at[order].set(dest_sorted)
    y = y_rows[dest].reshape(T, TOP_K, D) * gates[..., None].astype(h.dtype)
    return jnp.sum(y, axis=1)


def setup_inputs(seed: int = 0) -> dict:
    key = jax.random.key(seed)
    ks = jax.random.split(key, 24)
    f32 = jnp.float32

    def nrm(k, shape, s):
        return jax.random.normal(k, shape, f32) * s

    D = D_MODEL
    return {
        'x': nrm(ks[0], (BATCH, SEQ, D), 1.0),
        'c': nrm(ks[1], (BATCH, D), 1.0),
        'ctx': nrm(ks[2], (BATCH, CTX_LEN, D), 1.0),
        'c_ctx': nrm(ks[3], (D,), 1.0),
        'w_ada': nrm(ks[4], (DEPTH, D, 6 * D), 0.2 * D ** -0.5),
        'b_ada': nrm(ks[5], (DEPTH, 6 * D), 0.02),
        'g_attn': 1.0 + nrm(ks[6], (DEPTH, D), 0.02),
        'w_in': nrm(ks[7], (DEPTH, D, IN_COLS), D ** -0.5),
        'q_norm_g': 1.0 + nrm(ks[8], (DEPTH, HEAD_DIM), 0.02),
        'k_norm_g': 1.0 + nrm(ks[9], (DEPTH, HEAD_DIM), 0.02),
        'sink': nrm(ks[10], (DEPTH, N_HEADS), 0.5),
        'conv_w': nrm(ks[11], (DEPTH, CONV_SIZE, CONV_W), CONV_SIZE ** -0.5),
        'g_out_attn': 1.0 + nrm(ks[12], (DEPTH, ATTN_W), 0.02),
        'g_out_conv': 1.0 + nrm(ks[13], (DEPTH, CONV_W), 0.02),
        'w_out': nrm(ks[14], (DEPTH, MIX_W, D), MIX_W ** -0.5),
        'g_ffn': 1.0 + nrm(ks[15], (DEPTH, D), 0.02),
        'w_router': nrm(ks[16], (D, N_EXPERTS), D ** -0.5),
        'router_bias': nrm(ks[17], (N_EXPERTS,), 0.01),
        'w_exp_gate': nrm(ks[18], (DEPTH, N_EXPERTS, D, D_FF_EXPERT), D ** -0.5),
        'w_exp_up': nrm(ks[19], (DEPTH, N_EXPERTS, D, D_FF_EXPERT), D ** -0.5),
        'w_exp_down': nrm(ks[20], (DEPTH, N_EXPERTS, D_FF_EXPERT, D), D_FF_EXPERT ** -0.5),
    }


def reference(x, c, ctx, c_ctx, w_ada, b_ada, g_attn, w_in, q_norm_g, k_norm_g, sink, conv_w,
              g_out_attn, g_out_conv, w_out, g_ffn, w_router, router_bias,
              w_exp_gate, w_exp_up, w_exp_down):
    B, L, D = x.shape
    C = ctx.shape[1]
    rows = L // GRID_W
    row_id = jnp.repeat(jnp.arange(rows, dtype=jnp.float32), GRID_W)
    col_id = jnp.tile(jnp.arange(GRID_W, dtype=jnp.float32), rows)
    inv_freq = ROPE_THETA ** (-jnp.arange(0, ROPE_AXIS_DIM, 2, dtype=jnp.float32) / ROPE_AXIS_DIM)
    ang_row = row_id[:, None] * inv_freq[None, :]
    ang_col = col_id[:, None] * inv_freq[None, :]
    silu_c = jax.nn.silu(c)
    silu_cc = jax.nn.silu(c_ctx)
    xc = ctx

    for l in range(DEPTH):
        last = l == DEPTH - 1
        mod = (jnp.dot(silu_c, w_ada[l]) + b_ada[l]).reshape(B, 6, 1, D)
        mod_c = (jnp.dot(silu_cc, w_ada[l]) + b_ada[l]).reshape(6, D)

        h = modulate(rmsnorm(x, g_attn[l]), mod[:, 0], mod[:, 1])
        hc = modulate(rmsnorm(xc, g_attn[l]), mod_c[0], mod_c[1])
        q, k, v, u, b_gate, c_gate = split_in(jnp.dot(h, w_in[l]))
        if last:
            kv_c = jnp.dot(hc, w_in[l][:, ATTN_W:ATTN_W + 2 * KV_W])
            k_c, v_c = kv_c[..., :KV_W], kv_c[..., KV_W:]
        else:
            q_c, k_c, v_c, u_c, b_gate_c, c_gate_c = split_in(jnp.dot(hc, w_in[l]))
        k_c = rmsnorm(k_c.reshape(B, C, N_KV_HEADS, HEAD_DIM), k_norm_g[l])
        v_c = v_c.reshape(B, C, N_KV_HEADS, HEAD_DIM)
        q = rope_2d(rmsnorm(q.reshape(B, L, N_HEADS, HEAD_DIM), q_norm_g[l]), ang_row, ang_col)
        k = rope_2d(rmsnorm(k.reshape(B, L, N_KV_HEADS, HEAD_DIM), k_norm_g[l]), ang_row, ang_col)
        v = v.reshape(B, L, N_KV_HEADS, HEAD_DIM)
        attn = window_attention(q, k, v, k_c, v_c, sink[l])
        conv = b_gate * dwconv3(c_gate * u, conv_w[l])
        mixed = jnp.dot(jnp.concatenate([rmsnorm(attn, g_out_attn[l]),
                                         rmsnorm(conv, g_out_conv[l])], axis=-1), w_out[l])
        x = x + mod[:, 2] * mixed
        if not last:
            q_c = rmsnorm(q_c.reshape(B, C, N_HEADS, HEAD_DIM), q_norm_g[l])
            attn_c = context_attention(q_c, k_c, v_c, sink[l])
            conv_c = b_gate_c * dwconv3(c_gate_c * u_c, conv_w[l])
            mixed_c = jnp.dot(jnp.concatenate([rmsnorm(attn_c, g_out_attn[l]),
                                               rmsnorm(conv_c, g_out_conv[l])], axis=-1), w_out[l])
            xc = xc + mod_c[2] * mixed_c

        h2 = modulate(rmsnorm(x, g_ffn[l]), mod[:, 3], mod[:, 4])
        if last:
            y = grouped_moe(h2.reshape(B * L, D), w_router, router_bias,
                            w_exp_gate[l], w_exp_up[l], w_exp_down[l])
            x = x + mod[:, 5] * y.reshape(B, L, D)
        else:
            h2c = modulate(rmsnorm(xc, g_ffn[l]), mod_c[3], mod_c[4])
            tokens = jnp.concatenate([h2c.reshape(B * C, D), h2.reshape(B * L, D)], axis=0)
            y = grouped_moe(tokens, w_router, router_bias,
                            w_exp_gate[l], w_exp_up[l], w_exp_down[l])
            xc = xc + mod_c[5] * y[:B * C].reshape(B, C, D)
            x = x + mod[:, 5] * y[B * C:].reshape(B, L, D)
    return x
```

```python
import contextlib
import numpy as np
import concourse.bass as bass
import concourse.mybir as mybir
from concourse.bass_utils import run_bass_kernel_spmd

F32 = mybir.dt.float32
BF16 = mybir.dt.bfloat16
ALU = mybir.AluOpType
AF = mybir.ActivationFunctionType
AX = mybir.AxisListType

NCORES = 8
D = 2048
DC = 16
CTXL = 32
LAT = 1024
T = CTXL + LAT
NT = 9
TCH = [(0, 352), (352, 704), (704, 1056)]
IN_COLS = 4352
EPS = 1e-6
NEXP = 16
DFF = 1024

ENGS = ("pe", "act", "dve", "pool", "sp")
SAME_ENGINE_SYNC = True
DEBUG_SRC = False
N_DMA_SEMS = 12
SEM_EPOCH = 2000
DMA_EPOCH = 30


def tile_rows(t):
    return CTXL if t == 0 else 128


def tile_c0(t):
    return 0 if t == 0 else CTXL + 128 * (t - 1)


class _Op:
    __slots__ = ("eng", "fn", "deps", "needs_inc", "tok_sem", "tok_val", "is_dma", "pre_wait", "src")

    def __init__(self, eng, fn):
        self.eng = eng
        self.fn = fn
        self.deps = []
        self.needs_inc = False
        self.tok_sem = None
        self.tok_val = None
        self.is_dma = False
        self.pre_wait = None
        self.src = None
        if DEBUG_SRC:
            import sys as _s
            f = _s._getframe(2)
            ls = []
            while f is not None and len(ls) < 4:
                ls.append(f.f_lineno)
                f = f.f_back
            self.src = ls


class Prog:
    def __init__(self, nc):
        self.nc = nc
        self.ops = {e: [] for e in ENGS}
        self.last_w = {}
        self.readers = {}
        self.dma_count = {e: 0 for e in ENGS}
        self.dma_last = {}

    def _add_deps(self, op, reads, writes):
        deps = []
        for r in reads:
            w = self.last_w.get(r)
            if w is not None:
                deps.append(w)
        for r in writes:
            w = self.last_w.get(r)
            if w is not None:
                deps.append(w)
            rd = self.readers.get(r)
            if rd:
                deps.extend(rd[0].values())
                deps.extend(rd[1])
        seen = set()
        for d in deps:
            if d is op or id(d) in seen:
                continue
            seen.add(id(d))
            if not d.is_dma and d.eng == op.eng and not op.is_dma:
                if d.eng == "pe" or not SAME_ENGINE_SYNC:
                    continue
            if not d.is_dma:
                d.needs_inc = True
            op.deps.append(d)
        for r in reads:
            rd = self.readers.setdefault(r, ({}, []))
            if op.is_dma:
                rd[1].append(op)
            else:
                rd[0][op.eng] = op
        for r in writes:
            self.last_w[r] = op
            self.readers[r] = ({}, [])

    def op(self, eng, fn, reads=(), writes=()):
        o = _Op(eng, fn)
        self._add_deps(o, reads, writes)
        self.ops[eng].append(o)
        return o

    def dma(self, eng, out, in_, reads=(), writes=()):
        o = _Op(eng, lambda e: e.dma_start(out=out, in_=in_))
        o.is_dma = True
        i = self.dma_count[eng]
        self.dma_count[eng] = i + 1
        k = i % N_DMA_SEMS
        rnd = i // N_DMA_SEMS
        ep = rnd // DMA_EPOCH
        o.tok_sem = ("dma", eng, k, ep)
        o.tok_val = 16 * (rnd % DMA_EPOCH + 1)
        o.pre_wait = self.dma_last.get((eng, k, ep))
        self.dma_last[(eng, k, ep)] = o
        self._add_deps(o, reads, writes)
        self.ops[eng].append(o)
        return o

    def emit(self):
        nc = self.nc
        with contextlib.ExitStack() as st:
            sems = {}
            for e in ENGS:
                n_inc = sum(1 for o in self.ops[e] if (not o.is_dma) and o.needs_inc)
                for ep in range(n_inc // SEM_EPOCH + 1):
                    sems[("eng", e, ep)] = st.enter_context(nc.semaphore("s_%s%d" % (e, ep)))
                if self.dma_count[e]:
                    n_ep = (self.dma_count[e] - 1) // (N_DMA_SEMS * DMA_EPOCH) + 1
                    for ep in range(n_ep):
                        for k in range(N_DMA_SEMS):
                            sems[("dma", e, k, ep)] = st.enter_context(nc.semaphore("d_%s%d_%d" % (e, k, ep)))
            for e in ENGS:
                c = 0
                for o in self.ops[e]:
                    if o.is_dma:
                        continue
                    if o.needs_inc:
                        o.tok_sem = ("eng", e, c // SEM_EPOCH)
                        o.tok_val = c % SEM_EPOCH + 1
                        c += 1
            block = st.enter_context(nc.Block())
            ops = self.ops
            dma_last = self.dma_last

            def run(e, eng):
                seen = {}
                for o in ops[e]:
                    deps = list(o.deps)
                    if o.pre_wait is not None:
                        deps.append(o.pre_wait)
                    for d in deps:
                        if seen.get(d.tok_sem, 0) >= d.tok_val:
                            continue
                        seen[d.tok_sem] = d.tok_val
                        eng.wait_ge(sems[d.tok_sem], d.tok_val)
                    try:
                        ins = o.fn(eng)
                    except Exception:
                        print("FAILED OP created at lines", o.src)
                        raise
                    if o.is_dma:
                        ins.then_inc(sems[o.tok_sem], 16)
                    elif o.needs_inc:
                        ins.then_inc(sems[o.tok_sem], 1)
                for (ee, k, ep), o in dma_last.items():
                    if ee == e:
                        eng.wait_ge(sems[o.tok_sem], o.tok_val)

            @block.tensor
            def _(eng):
                run("pe", eng)

            @block.scalar
            def _(eng):
                run("act", eng)

            @block.vector
            def _(eng):
                run("dve", eng)

            @block.gpsimd
            def _(eng):
                run("pool", eng)

            @block.sync
            def _(eng):
                run("sp", eng)


def build_program(L, n_exp=NEXP, debug=False):
    nc = bass.Bass("TRN2", target_bir_lowering=False)
    P = Prog(nc)

    def din(name, shape, dt=F32):
        return nc.dram_tensor(name, list(shape), dt, kind="ExternalInput").ap()

    xT_d = din("xT", [D, T])
    ccT_d = din("ccT", [128, DC, 2])
    w_ada_d = din("w_ada", [L, D, 6 * D])
    badaT_d = din("badaT", [L, 128, 96])
    w_in_d = din("w_in", [L, D, IN_COLS])
    w_out_d = din("w_out", [L, D, D])
    NE_IN = max(n_exp, 1)
    wg_d = din("w_exp_gate", [L, NE_IN, D, DFF])
    wu_d = din("w_exp_up", [L, NE_IN, D, DFF])
    wd_d = din("w_exp_down", [L, NE_IN, DFF, D])
    pvec_d = din("pvec", [L, 128, 72])
    qkg_d = din("qkg", [L, 128, 128])
    sink_d = din("sinkb", [L, 128, 16])
    rbias_d = din("rbias", [128, 16])
    wr_d = din("w_routerT", [128, DC, 16])
    cos_d = din("ropec", [128, NT, 64])
    sin_d = din("ropes", [128, NT, 64])
    selkv_d = din("selkv", [128, 16])
    selcu_d = din("selcu", [32, 4])
    masks_d = din("masks", [128, 2, 128])
    out_d = nc.dram_tensor("outT", [D, LAT], F32, kind="ExternalOutput").ap()
    dbg_d = nc.dram_tensor("dbg", [D + 256, T], F32, kind="ExternalOutput").ap() if debug else None

    kv_loc = nc.dram_tensor("kv_loc", [T, 384], F32)
    kv_all = nc.dram_tensor("kv_all", [NCORES * T, 384], F32)
    cu_loc = nc.dram_tensor("cu_loc", [4, 1024], F32)
    cu_all = nc.dram_tensor("cu_all", [NCORES * 4, 1024], F32)

    st = contextlib.ExitStack()
    with st:
        def sb(name, shape, dt=F32):
            return st.enter_context(nc.sbuf_tensor(name, list(shape), dt))

        xa = sb("xarena", [128, DC * T])
        xT = xa[:].rearrange("p (k t) -> p k t", t=T)
        hT = sb("hTs", [128, DC, T], BF16)
        ident = sb("ident", [128, 128])
        ones_bf = sb("ones_bf", [128, 128], BF16)
        ones_f = sb("ones_f", [128, 128])
        masks = sb("masks_s", [128, 2, 128], BF16)
        selkv = sb("selkv_s", [128, 16])
        selcu = sb("selcu_s", [32, 4])
        ropec = sb("ropec_s", [128, NT, 64])
        ropes = sb("ropes_s", [128, NT, 64])
        siluc = sb("siluc", [128, DC, 2], BF16)
        ccT = sb("ccT_s", [128, DC, 2])
        modS = sb("modS", [128, 6, DC, 2])
        badaT = sb("badaT_s", [128, 96])
        pvec = sb("pvec_s", [128, 72])
        A1 = sb("A1", [128, DC, 2])
        A2 = sb("A2", [128, DC, 2])
        qkg = sb("qkg_s", [128, 128])
        sinkb = sb("sink_s", [128, 16])
        esink = sb("esink", [128, 16])
        rbias = sb("rbias_s", [128, 16])
        wrT = sb("wrT", [128, DC, 16])
        eps_t = sb("eps_t", [128, 1])
        fence_t = sb("fence_t", [128, 1])
        rstd_bc = sb("rstd_bc", [128, T])
        rstd_c_bc = sb("rstd_c_bc", [128, T])
        tmpf = sb("tmpf", [128, T])
        tmpf2 = sb("tmpf2", [128, T])
        sqb = sb("sqb", [128, T], BF16)
        NSLOT = 2
        wslot = [sb("wslot%d" % i, [128, DC, 512], BF16) for i in range(NSLOT)]
        bfa = sb("bfarena", [128, 16896], BF16)
        convgT = bfa[:, 0:8448].rearrange("p (k t) -> p k t", t=T)
        qT = bfa[:, 8448:16896].rearrange("p (k t) -> p k t", t=T)
        dslot = [bfa[:, 4096 * i:4096 * (i + 1)].rearrange("p (k c) -> p k c", c=D) for i in range(2)]
        hid = [bfa[:, 8192 + 2112 * i:8192 + 2112 * (i + 1)].rearrange("p (k t) -> p k t", t=T) for i in range(2)]
        s_sb = [sb("s_sb%d" % i, [128, 352]) for i in range(2)]
        rt = sb("rt", [128, 8, 16])
        eoh_e = sb("eoh_e", [16, 128])
        off = [0]

        def xf(n):
            o = off[0]
            off[0] += n
            return xa[:, o:o + n]

        def xb(n):
            return xf((n + 1) // 2).bitcast(BF16)

        kvb = [xf(384) for _ in range(2)]
        kvh = [xf(384) for _ in range(2)]
        v1h = [xf(130) for _ in range(2)]
        tokA, tokB, tokC, tokD = xf(512), xf(512), xf(512), xf(512)
        qr = [xf(512) for _ in range(2)]
        small = xf(64)
        kvt = xf(384)
        cu = xf(T + 4)
        b_sb = xf(T)
        u_sb = xf(352)
        cacc = xf(T)
        halo_sb = xf(32).rearrange("p (j f) -> p j f", f=4)
        cu_all_sb = xf(1024)
        cub = xf(1024)
        bcl = xf(128)
        ssqa = xf(NT)
        rstda = xf(NT)
        KT = xb(2 * 12 * 128).rearrange("p (a b c) -> p a b c", a=2, b=12)
        V1 = xb(12 * 2 * 65).rearrange("p (a b c) -> p a b c", a=12, b=2)
        PT = [xb(1024) for _ in range(2)]
        hTb = xb(64).rearrange("p (k c) -> p k c", c=4)
        assert off[0] <= DC * T, off[0]
        MIXK = ["kvb0", "kvb1", "kvh0", "kvh1", "v1h0", "v1h1", "tokA", "tokB", "tokC", "tokD", "qr0", "qr1", "small", "kvt",
                "cu", "b_sb", "u_sb", "cacc", "halo_sb", "cu_all_sb", "cub", "bcl", "ssqa", "rstda", "KT", "V1", "PT0", "PT1", "hTb"]
        BFK = ["convg%d" % j for j in range(8)] + ["qT", "dslot0", "dslot1", "hid0", "hid1"]
        ps = [st.enter_context(nc.psum_tensor("ps%d" % i, [128, 512], F32)) for i in range(8)]
        x_spill = nc.dram_tensor("x_spill", [D, T], F32)

        def MM(out, lhsT, rhs, start, stop, r, w):
            P.op("pe", lambda e: e.matmul(out, lhsT=lhsT, rhs=rhs, start=start, stop=stop), reads=r, writes=w)

        def TR(out, in_, idn, r, w):
            P.op("pe", lambda e: e.transpose(out, in_, idn), reads=r, writes=w)

        def ACT(out, in_, func, r, w, scale=1.0, bias=None):
            if bias is None:
                P.op("act", lambda e: e.activation(out=out, in_=in_, func=func, scale=scale), reads=r, writes=w)
            else:
                P.op("act", lambda e: e.activation(out=out, in_=in_, func=func, scale=scale, bias=bias), reads=r, writes=w)

        def TT(eng, out, in0, in1, op, r, w):
            P.op(eng, lambda e: e.tensor_tensor(out=out, in0=in0, in1=in1, op=op), reads=r, writes=w)

        def TS(eng, out, in0, s1, s2, op0, op1, r, w):
            if s2 is None:
                P.op(eng, lambda e: e.tensor_scalar(out=out, in0=in0, scalar1=s1, scalar2=None, op0=op0), reads=r, writes=w)
            else:
                P.op(eng, lambda e: e.tensor_scalar(out=out, in0=in0, scalar1=s1, scalar2=s2, op0=op0, op1=op1), reads=r, writes=w)

        def STT(eng, out, in0, scalar, in1, op0, op1, r, w):
            P.op(eng, lambda e: e.scalar_tensor_tensor(out=out, in0=in0, scalar=scalar, in1=in1, op0=op0, op1=op1), reads=r, writes=w)

        def CP(eng, out, in_, r, w):
            if eng == "act":
                P.op("act", lambda e: e.copy(out=out, in_=in_), reads=r, writes=w)
            else:
                P.op(eng, lambda e: e.tensor_copy(out=out, in_=in_), reads=r, writes=w)

        def RED(out, in_, op, r, w):
            P.op("dve", lambda e: e.tensor_reduce(out=out, in_=in_, axis=AX.X, op=op), reads=r, writes=w)

        def RECIP(out, in_, r, w):
            P.op("dve", lambda e: e.reciprocal(out=out, in_=in_), reads=r, writes=w)

        def MEMSET(eng, ap, val, w):
            P.op(eng, lambda e: e.memset(ap, val), writes=w)

        def LOAD(dst, src, w, eng="sp"):
            P.dma(eng, dst, src, writes=w)

        wctr = [0]

        def wload(parts):
            i = wctr[0] % NSLOT
            wctr[0] += 1
            key = "wslot%d" % i
            for (c0, n, src, kk) in parts:
                P.dma("pool", wslot[i][:, 0:kk, c0:c0 + n], src, writes=[key])
            return wslot[i], key

        def wsrc(mat2d, c0, n):
            return mat2d[:, c0:c0 + n].rearrange("(k p) c -> p k c", p=128)

        MEMSET("pool", ident[:], 1.0, ["ident"])
        P.op("pool", lambda e: e.affine_select(out=ident[:], in_=ident[:], pattern=[[-1, 128]],
                                                compare_op=ALU.is_equal, fill=0.0, base=0, channel_multiplier=1),
             reads=["ident"], writes=["ident"])
        MEMSET("dve", ones_bf[:], 1.0, ["ones_bf"])
        MEMSET("dve", ones_f[:], 1.0, ["ones_f"])
        MEMSET("dve", eps_t[:], EPS, ["eps_t"])
        P.dma("pool", masks[:], masks_d, writes=["masks"])
        LOAD(selkv[:], selkv_d, ["selkv"])
        LOAD(selcu[:], selcu_d, ["selcu"])
        LOAD(ropec[:], cos_d, ["ropec"])
        LOAD(ropes[:], sin_d, ["ropes"])
        LOAD(ccT[:], ccT_d, ["ccT"])
        LOAD(rbias[:], rbias_d, ["rbias"])
        LOAD(wrT[:], wr_d, ["wrT"])
        MEMSET("dve", fence_t[:], 0.0, ["fence_t"])

        def fence(keys):
            P.op("dve", lambda e: e.memset(fence_t[:], 0.0), writes=list(keys) + ["fence_t"])

        ACT(siluc[:], ccT[:], AF.Silu, ["ccT"], ["siluc"])
        for k in range(DC):
            LOAD(xT[:, k, :], xT_d[k * 128:(k + 1) * 128, :], ["x%d" % k])

        XK = ["x%d" % k for k in range(DC)]
        HK = ["h%d" % k for k in range(DC)]

        for l in range(L):
            LOAD(badaT[:], badaT_d[l], ["badaT"])
            LOAD(pvec[:], pvec_d[l], ["pvec"])
            LOAD(qkg[:], qkg_d[l], ["qkg"])
            LOAD(sinkb[:], sink_d[l], ["sinkb"])
            ACT(esink[:], sinkb[:], AF.Exp, ["sinkb"], ["esink"])
            g_attn = pvec[:, 0:16]
            g_ffn = pvec[:, 16:32]
            g_oa = pvec[:, 32:40]
            g_oc = pvec[:, 40:48]
            cw = pvec[:, 48:72]

            modps = ps[7][:, 0:192]
            for u in range(24):
                ws, wk = wload([(0, 512, wsrc(w_ada_d[l], u * 512, 512), DC)])
                pa = ps[u % 2]
                for k in range(DC):
                    MM(pa[0:2, :], siluc[:, k, :], ws[:, k, :], k == 0, k == DC - 1, ["siluc", wk], ["ps%d" % (u % 2)])
                CP("act", tmpf[0:2, 0:512], pa[0:2, :], ["ps%d" % (u % 2)], ["tmpf"])
                for s_ in range(4):
                    j = u * 4 + s_
                    TR(ps[7][:, 2 * j:2 * j + 2], tmpf[0:2, s_ * 128:(s_ + 1) * 128], ident[0:2, 0:2], ["tmpf", "ident"], ["ps7"])
            TT("dve", modS[:].rearrange("p a k v -> p (a k) v"), modps.rearrange("p (j v) -> p j v", v=2),
               badaT[:, :, None].broadcast_to([128, 96, 2]), ALU.add, ["ps7", "badaT"], ["modS"])
            for v in range(2):
                STT("dve", A1[:, :, v], modS[:, 1, :, v], 1.0, g_attn, ALU.add, ALU.mult, ["modS", "pvec"], ["A1"])
                STT("dve", A2[:, :, v], modS[:, 4, :, v], 1.0, g_ffn, ALU.add, ALU.mult, ["modS", "pvec"], ["A2"])

            def norm_mod(Amat, shift_idx, out_f32_cb=None):
                for k in range(DC):
                    ACT(sqb[:], xT[:, k, :], AF.Square, ["x%d" % k], ["sqb"])
                    for ti, (a, b) in enumerate(TCH):
                        MM(ps[4 + ti][:, 0:352], ones_bf[:], sqb[:, a:b], k == 0, k == DC - 1, ["ones_bf", "sqb"], ["ps%d" % (4 + ti)])
                for ti, (a, b) in enumerate(TCH):
                    ACT(rstd_bc[:, a:b], ps[4 + ti][:, 0:352], AF.Sqrt, ["ps%d" % (4 + ti), "eps_t"], ["rstd_bc"], scale=1.0 / D, bias=eps_t[:, 0:1])
                RECIP(rstd_bc[:], rstd_bc[:], ["rstd_bc"], ["rstd_bc"])
                for k in range(DC):
                    TT("dve", tmpf[:], xT[:, k, :], rstd_bc[:], ALU.mult, ["x%d" % k, "rstd_bc"], ["tmpf"])
                    if out_f32_cb is None:
                        ACT(hT[:, k, 0:CTXL], tmpf[:, 0:CTXL], AF.Identity, ["tmpf", "A1", "A2", "modS"], ["h%d" % k],
                            scale=Amat[:, k, 1:2], bias=modS[:, shift_idx, k, 1:2])
                        ACT(hT[:, k, CTXL:T], tmpf[:, CTXL:T], AF.Identity, ["tmpf", "A1", "A2", "modS"], ["h%d" % k],
                            scale=Amat[:, k, 0:1], bias=modS[:, shift_idx, k, 0:1])
                    else:
                        ACT(tmpf2[:, 0:CTXL], tmpf[:, 0:CTXL], AF.Identity, ["tmpf", "A1", "A2", "modS"], ["tmpf2"],
                            scale=Amat[:, k, 1:2], bias=modS[:, shift_idx, k, 1:2])
                        ACT(tmpf2[:, CTXL:T], tmpf[:, CTXL:T], AF.Identity, ["tmpf", "A1", "A2", "modS"], ["tmpf2"],
                            scale=Amat[:, k, 0:1], bias=modS[:, shift_idx, k, 0:1])
                        CP("pool", hT[:, k, :], tmpf2[:], ["tmpf2"], ["h%d" % k])
                        out_f32_cb(k)

            norm_mod(A1, 0)
            for k in range(DC):
                P.dma("sp", x_spill.ap()[k * 128:(k + 1) * 128, :], xT[:, k, :], reads=["x%d" % k], writes=["xsp%d" % k])
            fence(XK + MIXK + BFK)

            def qk_norm_rope(src_ps, rows, nh, gcol0, tile, dst, dst_keys, psk):
                n = nh * 64
                ACT(tokA[:rows, 0:n], src_ps, AF.Square, [psk], ["tokA"])
                RED(small[:rows, 0:nh], tokA[:rows, 0:n].rearrange("p (h d) -> p h d", d=64), ALU.add, ["tokA"], ["small"])
                ACT(small[:rows, 0:nh], small[:rows, 0:nh], AF.Sqrt, ["small", "eps_t"], ["small"], scale=1.0 / 64, bias=eps_t[:rows, 0:1])
                RECIP(small[:rows, 0:nh], small[:rows, 0:nh], ["small"], ["small"])
                TT("dve", tokB[:rows, 0:n].rearrange("p (h d) -> p h d", d=64), src_ps.rearrange("p (h d) -> p h d", d=64),
                   small[:rows, 0:nh, None].broadcast_to([rows, nh, 64]), ALU.mult, [psk, "small"], ["tokB"])
                TT("pool", tokB[:rows, 0:n].rearrange("p (h d) -> p h d", d=64), tokB[:rows, 0:n].rearrange("p (h d) -> p h d", d=64),
                   qkg[:rows, None, gcol0:gcol0 + 64].broadcast_to([rows, nh, 64]), ALU.mult, ["tokB", "qkg"], ["tokB"])
                v4 = tokB[:rows, 0:n].rearrange("p (a s d) -> p a s d", s=2, d=16)
                w4 = tokC[:rows, 0:n].rearrange("p (a s d) -> p a s d", s=2, d=16)
                CP("pool", w4[:, :, 0, :], v4[:, :, 1, :], ["tokB"], ["tokC"])
                CP("pool", w4[:, :, 1, :], v4[:, :, 0, :], ["tokB"], ["tokC"])
                TT("dve", tokB[:rows, 0:n].rearrange("p (h d) -> p h d", d=64), tokB[:rows, 0:n].rearrange("p (h d) -> p h d", d=64),
                   ropec[:rows, tile, None, :].broadcast_to([rows, nh, 64]), ALU.mult, ["tokB", "ropec"], ["tokB"])
                TT("pool", tokC[:rows, 0:n].rearrange("p (h d) -> p h d", d=64), tokC[:rows, 0:n].rearrange("p (h d) -> p h d", d=64),
                   ropes[:rows, tile, None, :].broadcast_to([rows, nh, 64]), ALU.mult, ["tokC", "ropes"], ["tokC"])
                TT("dve", dst, tokB[:rows, 0:n], tokC[:rows, 0:n], ALU.add, ["tokB", "tokC"], dst_keys)

            ws, wk = wload([(0, 256, wsrc(w_in_d[l], 1024, 256), DC)])
            for t in range(NT):
                rows, c0 = tile_rows(t), tile_c0(t)
                pk = t % 2
                for k in range(DC):
                    MM(ps[pk][:rows, 0:256], hT[:, k, c0:c0 + rows], ws[:, k, 0:256], k == 0, k == DC - 1, ["h%d" % k, wk], ["ps%d" % pk])
                qk_norm_rope(ps[pk][:rows, 0:128], rows, 2, 64, t, tokD[:rows, 0:128], ["tokD"], "ps%d" % pk)
                kv4 = kvt[:rows, 0:256].rearrange("p (h s d) -> p h s d", s=2, d=64)
                kd = tokD[:rows, 0:128].rearrange("p (h d) -> p h d", d=64)
                CP("dve", kv4[:, :, 0, :], kd, ["tokD"], ["kvt"])
                CP("pool", kv4[:, :, 1, :], kd, ["tokD"], ["kvt"])
                CP("act", kvt[:rows, 256:384], ps[pk][:rows, 128:256], ["ps%d" % pk], ["kvt"])
                P.dma("sp", kv_loc.ap()[c0:c0 + rows, :], kvt[:rows, :], reads=["kvt"], writes=["kv_loc"])
            P.op("pool", lambda e: e.collective_compute("AllGather", ALU.bypass, replica_groups=[list(range(NCORES))],
                                                         ins=[kv_loc.ap().opt()], outs=[kv_all.ap().opt()]),
                 reads=["kv_loc"], writes=["kv_all"])

            for i, col in enumerate((0, CTXL - 1, CTXL, T - 1)):
                CP("dve", hTb[:, :, i:i + 1], hT[:, :, col:col + 1], HK, ["hTb"])
            for i in range(2):
                ws, wk = wload([(0, 512, wsrc(w_in_d[l], 1280 + 512 * i, 512), DC)])
                ws2, wk2 = wload([(0, 512, wsrc(w_in_d[l], 3328 + 512 * i, 512), DC)])
                for k in range(DC):
                    MM(ps[0][0:4, :], hTb[:, k, :], ws[:, k, :], k == 0, k == DC - 1, ["hTb", wk], ["ps0"])
                for k in range(DC):
                    MM(ps[1][0:4, :], hTb[:, k, :], ws2[:, k, :], k == 0, k == DC - 1, ["hTb", wk2], ["ps1"])
                CP("act", tokA[0:4, :], ps[0][0:4, :], ["ps0"], ["tokA"])
                TT("dve", cub[0:4, 512 * i:512 * (i + 1)], tokA[0:4, :], ps[1][0:4, :], ALU.mult, ["tokA", "ps1"], ["cub"])
            P.dma("sp", cu_loc.ap(), cub[0:4, :], reads=["cub"], writes=["cu_loc"])
            P.op("pool", lambda e: e.collective_compute("AllGather", ALU.bypass, replica_groups=[list(range(NCORES))],
                                                         ins=[cu_loc.ap().opt()], outs=[cu_all.ap().opt()]),
                 reads=["cu_loc"], writes=["cu_all"])
            P.dma("sp", cu_all_sb[0:32, :], cu_all.ap(), reads=["cu_all"], writes=["cu_all_sb"])
            for j in range(8):
                MM(ps[2][:, 4 * j:4 * j + 4], cu_all_sb[0:32, j * 128:(j + 1) * 128], selcu[:], True, True, ["cu_all_sb", "selcu"], ["ps2"])
            CP("dve", halo_sb[:].rearrange("p j f -> p (j f)"), ps[2][:, 0:32], ["ps2"], ["halo_sb"])

            def cucol(tok):
                return tok + 1 if tok < CTXL else tok + 3

            rot = [0]
            for j in range(8):
                ws, wk = wload([(0, 128, wsrc(w_in_d[l], 1280 + 128 * j, 128), DC),
                                (128, 128, wsrc(w_in_d[l], 2304 + 128 * j, 128), DC),
                                (256, 128, wsrc(w_in_d[l], 3328 + 128 * j, 128), DC)])
                for ti, (a, b) in enumerate(TCH):
                    pp = []
                    for part in range(3):
                        pi = rot[0] % 4
                        rot[0] += 1
                        pp.append(pi)
                        for k in range(DC):
                            MM(ps[pi][:, 0:352], ws[:, k, part * 128:(part + 1) * 128], hT[:, k, a:b], k == 0, k == DC - 1,
                               ["h%d" % k, wk], ["ps%d" % pi])
                    pu, pb, pc = pp
                    CP("act", u_sb[:], ps[pu][:, 0:352], ["ps%d" % pu], ["u_sb"])
                    if a == 0:
                        TT("dve", cu[:, 1:1 + CTXL], u_sb[:, 0:CTXL], ps[pc][:, 0:CTXL], ALU.mult, ["u_sb", "ps%d" % pc], ["cu"])
                        TT("dve", cu[:, 35:35 + 352 - CTXL], u_sb[:, CTXL:352], ps[pc][:, CTXL:352], ALU.mult, ["u_sb", "ps%d" % pc], ["cu"])
                    else:
                        TT("dve", cu[:, a + 3:b + 3], u_sb[:], ps[pc][:, 0:352], ALU.mult, ["u_sb", "ps%d" % pc], ["cu"])
                    CP("act", b_sb[:, a:b], ps[pb][:, 0:352], ["ps%d" % pb], ["b_sb"])
                for hi, col in enumerate((0, 33, 34, T + 3)):
                    CP("pool", cu[:, col:col + 1], halo_sb[:, j, hi:hi + 1], ["halo_sb"], ["cu"])
                for (base, n, t0) in ((1, CTXL, 0), (35, LAT, CTXL)):
                    TS("pool", cacc[:, t0:t0 + n], cu[:, base - 1:base - 1 + n], cw[:, 3 * j:3 * j + 1], None, ALU.mult, None, ["cu", "pvec"], ["cacc"])
                    STT("dve", cacc[:, t0:t0 + n], cu[:, base:base + n], cw[:, 3 * j + 1:3 * j + 2], cacc[:, t0:t0 + n], ALU.mult, ALU.add, ["cu", "pvec", "cacc"], ["cacc"])
                    STT("dve", cacc[:, t0:t0 + n], cu[:, base + 1:base + 1 + n], cw[:, 3 * j + 2:3 * j + 3], cacc[:, t0:t0 + n], ALU.mult, ALU.add, ["cu", "pvec", "cacc"], ["cacc"])
                TT("pool", cacc[:], cacc[:], b_sb[:], ALU.mult, ["cacc", "b_sb"], ["cacc"])
                ACT(convgT[:, j, :], cacc[:], AF.Copy, ["cacc", "pvec"], ["convg%d" % j], scale=g_oc[:, j:j + 1])
                ACT(sqb[:], cacc[:], AF.Square, ["cacc"], ["sqb"])
                for ti, (a, b) in enumerate(TCH):
                    MM(ps[4 + ti][:, 0:352], ones_bf[:], sqb[:, a:b], j == 0, j == 7, ["ones_bf", "sqb"], ["ps%d" % (4 + ti)])
            for ti, (a, b) in enumerate(TCH):
                ACT(rstd_c_bc[:, a:b], ps[4 + ti][:, 0:352], AF.Sqrt, ["ps%d" % (4 + ti), "eps_t"], ["rstd_c_bc"], scale=1.0 / 1024, bias=eps_t[:, 0:1])
            RECIP(rstd_c_bc[:], rstd_c_bc[:], ["rstd_c_bc"], ["rstd_c_bc"])

            kva = kv_all.ap().rearrange("(r t) c -> r t c", t=T)
            MEMSET("dve", V1[:, :, :, 64:65], 1.0, ["V1"])
            for bi in range(10):
                kb = bi + 1 if bi < 8 else bi + 2
                kbuf = kvb[bi % 2]
                kk_ = "kvb%d" % (bi % 2)
                if bi < 8:
                    P.dma("sp", kbuf[:, :], kv_loc.ap()[CTXL + 128 * bi:CTXL + 128 * (bi + 1), :], reads=["kv_loc"], writes=[kk_])
                else:
                    for r4 in range(4):
                        P.dma("sp", kbuf[r4 * 32:(r4 + 1) * 32, :], kva[(bi - 8) * 4 + r4, 0:CTXL, :], reads=["kv_all"], writes=[kk_])
                for kv in range(2):
                    TR(ps[kv][:, 0:128], kbuf[:, kv * 128:(kv + 1) * 128], ident[:], [kk_, "ident"], ["ps%d" % kv])
                    CP("act" if kv else "dve", KT[:, kv, kb, :], ps[kv][:, 0:128], ["ps%d" % kv], ["KT"])
                CP("pool", V1[:, kb, :, 0:64], kbuf[:, 256:384].rearrange("p (h d) -> p h d", d=64), [kk_], ["V1"])
            for side in range(2):
                kb = 0 if side == 0 else 9
                r0 = T - 128 if side == 0 else CTXL
                for r_ in range(NCORES):
                    hb = kvh[r_ % 2]
                    hk_ = "kvh%d" % (r_ % 2)
                    vb = v1h[r_ % 2]
                    vk_ = "v1h%d" % (r_ % 2)
                    sc = selkv[:, side * 8 + r_:side * 8 + r_ + 1]
                    P.dma("sp", hb[:, :], kva[r_, r0:r0 + 128, :], reads=["kv_all"], writes=[hk_])
                    TS("dve", hb[:, :], hb[:, :], sc, None, ALU.mult, None, [hk_, "selkv"], [hk_])
                    v3 = vb[:, :].rearrange("p (h d) -> p h d", d=65)
                    CP("pool", v3[:, :, 0:64], hb[:, 256:384].rearrange("p (h d) -> p h d", d=64), [hk_], [vk_])
                    for h_ in range(2):
                        CP("pool", v3[:, h_, 64:65], sc, ["selkv"], [vk_])
                    for kv in range(2):
                        MM(ps[kv][:, 0:128], hb[:, kv * 128:(kv + 1) * 128], ident[:], r_ == 0, r_ == NCORES - 1, [hk_, "ident"], ["ps%d" % kv])
                    MM(ps[2][:, 0:130], ident[:], vb[:, :], r_ == 0, r_ == NCORES - 1, [vk_, "ident"], ["ps2"])
                for kv in range(2):
                    CP("act" if kv else "dve", KT[:, kv, kb, :], ps[kv][:, 0:128], ["ps%d" % kv], ["KT"])
                CP("dve", V1[:, kb, :, :], ps[2][:, 0:130].rearrange("p (h d) -> p h d", d=65), ["ps2"], ["V1"])

            MEMSET("dve", ssqa[:, :], 0.0, ["ssqa"])
            for g in range(2):
                ws, wk = wload([(0, 512, wsrc(w_in_d[l], 512 * g, 512), DC)])
                for t in range(NT):
                    rows, c0 = tile_rows(t), tile_c0(t)
                    pk = t % 2
                    qb = qr[t % 2]
                    qk_ = "qr%d" % (t % 2)
                    for k in range(DC):
                        MM(ps[pk][:rows, 0:512], hT[:, k, c0:c0 + rows], ws[:, k, 0:512], k == 0, k == DC - 1, ["h%d" % k, wk], ["ps%d" % pk])
                    qk_norm_rope(ps[pk][:rows, 0:512], rows, 8, 0, t, qb[:rows, 0:512], [qk_], "ps%d" % pk)
                    for pr in range(4):
                        TR(ps[2][:, pr * 128:pr * 128 + rows], qb[:rows, pr * 128:(pr + 1) * 128], ident[:rows, :rows], [qk_, "ident"], ["ps2"])
                    CP("act", qT[:, 4 * g:4 * g + 4, c0:c0 + rows], ps[2][:, :].rearrange("p (a q) -> p a q", q=128)[:, :, 0:rows], ["ps2"], ["qT"])
            for g in range(2):
                for t in range(NT):
                    rows, c0 = tile_rows(t), tile_c0(t)
                    if t == 0:
                        kbs = [(10, None), (11, None)]
                    else:
                        kbs = [(t - 1, 0), (t, None), (t + 1, 1), (10, None), (11, None)]
                    for ki, (kb, mk) in enumerate(kbs):
                        sp0 = 4 + 2 * (ki % 2)
                        for half in range(2):
                            MM(ps[sp0 + half][:, 0:4 * rows].rearrange("p (a q) -> p a q", q=rows),
                               KT[half * 64:(half + 1) * 64, g, kb, :], qT[half * 64:(half + 1) * 64, 4 * g:4 * g + 4, c0:c0 + rows],
                               True, True, ["KT", "qT"], ["ps%d" % (sp0 + half)])
                        pt = PT[ki % 2]
                        ptk = "PT%d" % (ki % 2)
                        for half in range(2):
                            ACT(pt[:, half * 512:half * 512 + 4 * rows], ps[sp0 + half][:, 0:4 * rows], AF.Exp,
                                ["ps%d" % (sp0 + half)], [ptk], scale=0.125)
                        if mk is not None:
                            for half in range(2):
                                v3 = pt[:, half * 512:half * 512 + 4 * rows].rearrange("p (a q) -> p a q", q=rows)
                                TT("pool", v3, v3, masks[:, mk, None, 0:rows].broadcast_to([128, 4, rows]), ALU.mult, [ptk, "masks"], [ptk])
                        for s_ in range(8):
                            ob = 0 + s_ // 4
                            MM(ps[ob][:rows, (s_ % 4) * 128:(s_ % 4) * 128 + 65],
                               pt[:, (s_ // 4) * 512 + (s_ % 4) * rows:(s_ // 4) * 512 + (s_ % 4) * rows + rows],
                               V1[:, kb, g, :], ki == 0 and s_ % 4 == 0, ki == len(kbs) - 1 and s_ % 4 == 3, [ptk, "V1"], ["ps%d" % ob])
                    for half in range(2):
                        o3 = ps[half][:rows, :].rearrange("p (a d) -> p a d", d=128)
                        es = esink[:rows, 8 * g:8 * g + 8].rearrange("p (a s) -> p a s", s=2)[:, :, half:half + 1]
                        TT("dve", small[:rows, 16 + 4 * half:20 + 4 * half, None], o3[:, :, 64:65], es, ALU.add, ["ps%d" % half, "esink"], ["small"])
                    RECIP(small[:rows, 16:24], small[:rows, 16:24], ["small"], ["small"])
                    for half in range(2):
                        o3 = ps[half][:rows, :].rearrange("p (a d) -> p a d", d=128)
                        dst = tokD[:rows, :].rearrange("p (a s d) -> p a s d", s=2, d=64)[:, :, half, :]
                        TT("dve", dst, o3[:, :, 0:64], small[:rows, 16 + 4 * half:20 + 4 * half, None].broadcast_to([rows, 4, 64]), ALU.mult,
                           ["ps%d" % half, "small"], ["tokD"])
                    ACT(tokA[:rows, :], tokD[:rows, :], AF.Square, ["tokD"], ["tokA"])
                    RED(small[:rows, 32:33], tokA[:rows, :], ALU.add, ["tokA"], ["small"])
                    TT("dve", ssqa[:rows, t:t + 1], ssqa[:rows, t:t + 1], small[:rows, 32:33], ALU.add, ["ssqa", "small"], ["ssqa"])
                    for pr in range(4):
                        TR(ps[2][:, pr * 128:pr * 128 + rows], tokD[:rows, pr * 128:(pr + 1) * 128], ident[:rows, :rows], ["tokD", "ident"], ["ps2"])
                    for pr in range(4):
                        ACT(hT[:, 4 * g + pr, c0:c0 + rows], ps[2][:, pr * 128:pr * 128 + rows], AF.Copy, ["ps2", "pvec"], ["h%d" % (4 * g + pr)],
                            scale=g_oa[:, 4 * g + pr:4 * g + pr + 1])
            ACT(rstda[:], ssqa[:], AF.Sqrt, ["ssqa", "eps_t"], ["rstda"], scale=1.0 / 1024, bias=eps_t[:, 0:1])
            RECIP(rstda[:], rstda[:], ["rstda"], ["rstda"])
            for t in range(NT):
                rows, c0 = tile_rows(t), tile_c0(t)
                TS("dve", bcl[:rows, :], ones_f[:rows, :], rstda[:rows, t:t + 1], None, ALU.mult, None, ["ones_f", "rstda"], ["bcl"])
                MM(ps[3][:, 0:rows], bcl[:rows, :], ident[:rows, :rows], True, True, ["bcl", "ident"], ["ps3"])
                CP("act", rstd_bc[:, c0:c0 + rows], ps[3][:, 0:rows], ["ps3"], ["rstd_bc"])

            if debug == 2 and l == L - 1:
                for k in range(8):
                    P.dma("pool", dbg_d[k * 128:(k + 1) * 128, :], hT[:, k, :], reads=["h%d" % k])
                    P.dma("pool", dbg_d[1024 + k * 128:1024 + (k + 1) * 128, :], convgT[:, k, :], reads=["convg%d" % k])
                P.dma("sp", dbg_d[2048:2176, :], rstd_bc[:], reads=["rstd_bc"])
                P.dma("sp", dbg_d[2176:2304, :], rstd_c_bc[:], reads=["rstd_c_bc"])
            fence(XK + MIXK)
            for k in range(DC):
                P.dma("sp", xT[:, k, :], x_spill.ap()[k * 128:(k + 1) * 128, :], reads=["xsp%d" % k], writes=["x%d" % k])
            rot = [0]
            for u in range(4):
                ws, wk = wload([(0, 512, wsrc(w_out_d[l], 512 * u, 512), DC)])
                for sub in range(4):
                    dk = 4 * u + sub
                    for ti, (a, b) in enumerate(TCH):
                        pa = rot[0] % 4
                        pc = 4 + rot[0] % 4
                        rot[0] += 1
                        for k in range(8):
                            MM(ps[pa][:, 0:352], ws[:, k, sub * 128:(sub + 1) * 128], hT[:, k, a:b], k == 0, k == 7, ["h%d" % k, wk], ["ps%d" % pa])
                        for k in range(8):
                            MM(ps[pc][:, 0:352], ws[:, 8 + k, sub * 128:(sub + 1) * 128], convgT[:, k, a:b], k == 0, k == 7, ["convg%d" % k, wk], ["ps%d" % pc])
                        TT("dve", tmpf[:, a:b], ps[pa][:, 0:352], rstd_bc[:, a:b], ALU.mult, ["ps%d" % pa, "rstd_bc"], ["tmpf"])
                        TT("dve", tmpf2[:, a:b], ps[pc][:, 0:352], rstd_c_bc[:, a:b], ALU.mult, ["ps%d" % pc, "rstd_c_bc"], ["tmpf2"])
                        TT("pool", tmpf[:, a:b], tmpf[:, a:b], tmpf2[:, a:b], ALU.add, ["tmpf", "tmpf2"], ["tmpf"])
                        if a == 0:
                            STT("dve", xT[:, dk, 0:CTXL], tmpf[:, 0:CTXL], modS[:, 2, dk, 1:2], xT[:, dk, 0:CTXL], ALU.mult, ALU.add,
                                ["tmpf", "modS", "x%d" % dk], ["x%d" % dk])
                            STT("dve", xT[:, dk, CTXL:b], tmpf[:, CTXL:b], modS[:, 2, dk, 0:1], xT[:, dk, CTXL:b], ALU.mult, ALU.add,
                                ["tmpf", "modS", "x%d" % dk], ["x%d" % dk])
                        else:
                            STT("dve", xT[:, dk, a:b], tmpf[:, a:b], modS[:, 2, dk, 0:1], xT[:, dk, a:b], ALU.mult, ALU.add,
                                ["tmpf", "modS", "x%d" % dk], ["x%d" % dk])

            if debug == 1 and l == L - 1 and n_exp == 0:
                for k in range(DC):
                    P.dma("sp", dbg_d[k * 128:(k + 1) * 128, :], xT[:, k, :], reads=["x%d" % k])

            if n_exp > 0:
                def router_cb(k):
                    for ti, (a, b) in enumerate(TCH):
                        MM(ps[ti][0:16, 0:352], wrT[:, k, :], tmpf2[:, a:b], k == 0, k == DC - 1, ["wrT", "tmpf2"], ["ps%d" % ti])

                norm_mod(A2, 3, router_cb)
                for ti, (a, b) in enumerate(TCH):
                    CP("act", tmpf[0:16, a:b], ps[ti][0:16, 0:352], ["ps%d" % ti], ["tmpf"])
                for t in range(NT):
                    rows, c0 = tile_rows(t), tile_c0(t)
                    TR(ps[3][:rows, 0:16], tmpf[0:16, c0:c0 + rows], ident[0:16, 0:16], ["tmpf", "ident"], ["ps3"])
                    S_ = rt[:rows, 0, :]
                    sel = rt[:rows, 1, :]
                    eq = rt[:rows, 2, :]
                    sel2 = rt[:rows, 3, :]
                    m1 = rt[:rows, 4, 0:4]
                    m2 = rt[:rows, 4, 4:8]
                    gs = rt[:rows, 4, 8:12]
                    gm = rt[:rows, 4, 12:13]
                    den = rt[:rows, 4, 13:14]
                    gmask = rt[:rows, 5, 0:4]
                    t2m = rt[:rows, 6, :]
                    Gm = rt[:rows, 7, :]
                    g44 = lambda ap: ap.rearrange("p (g e) -> p g e", e=4)
                    bc4 = lambda ap: ap[:, :, None].broadcast_to([rows, 4, 4])
                    ACT(S_, ps[3][:rows, 0:16], AF.Sigmoid, ["ps3"], ["rt"])
                    TT("dve", sel, S_, rbias[:rows, :], ALU.add, ["rt", "rbias"], ["rt"])
                    RED(m1, g44(sel), ALU.max, ["rt"], ["rt"])
                    TT("dve", g44(eq), g44(sel), bc4(m1), ALU.is_equal, ["rt"], ["rt"])
                    STT("dve", sel2, eq, -1.0e9, sel, ALU.mult, ALU.add, ["rt"], ["rt"])
                    RED(m2, g44(sel2), ALU.max, ["rt"], ["rt"])
                    TT("dve", gs, m1, m2, ALU.add, ["rt"], ["rt"])
                    RED(gm, gs, ALU.max, ["rt"], ["rt"])
                    TS("dve", gmask, gs, gm, None, ALU.is_equal, None, ["rt"], ["rt"])
                    TT("dve", g44(t2m), g44(sel), bc4(m2), ALU.is_ge, ["rt"], ["rt"])
                    TT("dve", g44(t2m), g44(t2m), bc4(gmask), ALU.mult, ["rt"], ["rt"])
                    TT("dve", t2m, t2m, S_, ALU.mult, ["rt"], ["rt"])
                    RED(den, t2m, ALU.add, ["rt"], ["rt"])
                    RECIP(den, den, ["rt"], ["rt"])
                    TS("dve", Gm, t2m, den, None, ALU.mult, None, ["rt"], ["rt"])
                    TR(ps[3][0:16, 128:128 + rows], Gm, ident[:rows, :rows], ["rt", "ident"], ["ps3"])
                    CP("act", tmpf2[0:16, c0:c0 + rows], ps[3][0:16, 128:128 + rows], ["ps3"], ["tmpf2"])

                fence(BFK)
                rot = [0]
                yrot = [0]
                units = [(e_, qf) for e_ in range(n_exp) for qf in range(4)]

                def load_unit(i):
                    e_, qf = units[i]
                    ws, wk = wload([(0, 256, wsrc(wg_d[l, e_], 256 * qf, 256), DC),
                                    (256, 256, wsrc(wu_d[l, e_], 256 * qf, 256), DC)])
                    di = i % 2
                    dk_ = "dslot%d" % di
                    P.dma("pool", dslot[di], wd_d[l, e_][256 * qf:256 * qf + 256, :].rearrange("(k p) c -> p k c", p=128), writes=[dk_])
                    return ws, wk, di, dk_

                loaded = {0: load_unit(0)}
                for i, (e_, qf) in enumerate(units):
                    if i + 1 < len(units):
                        loaded[i + 1] = load_unit(i + 1)
                    ws, wk, di, dk_ = loaded.pop(i)
                    if qf == 0:
                        TS("dve", eoh_e[:], ones_f[0:16, :], ident[0:16, e_:e_ + 1], None, ALU.mult, None, ["ones_f", "ident"], ["eoh_e"])
                        for ti, (a, b) in enumerate(TCH):
                            MM(ps[7][:, 0:352], eoh_e[:], tmpf2[0:16, a:b], True, True, ["eoh_e", "tmpf2"], ["ps7"])
                            CP("act", rstd_c_bc[:, a:b], ps[7][:, 0:352], ["ps7"], ["rstd_c_bc"])
                    hd = hid[i % 2]
                    hk = "hid%d" % (i % 2)
                    for fs in range(2):
                        for ti, (a, b) in enumerate(TCH):
                            pa = 2 * (rot[0] % 2)
                            pb = pa + 1
                            si = rot[0] % 2
                            rot[0] += 1
                            for k in range(DC):
                                MM(ps[pa][:, 0:352], ws[:, k, fs * 128:(fs + 1) * 128], hT[:, k, a:b], k == 0, k == DC - 1, ["h%d" % k, wk], ["ps%d" % pa])
                            for k in range(DC):
                                MM(ps[pb][:, 0:352], ws[:, k, 256 + fs * 128:256 + (fs + 1) * 128], hT[:, k, a:b], k == 0, k == DC - 1, ["h%d" % k, wk], ["ps%d" % pb])
                            ACT(s_sb[si][:], ps[pa][:, 0:352], AF.Silu, ["ps%d" % pa], ["s_sb%d" % si])
                            TT("dve", s_sb[si][:], s_sb[si][:], ps[pb][:, 0:352], ALU.mult, ["s_sb%d" % si, "ps%d" % pb], ["s_sb%d" % si])
                            TT("dve", hd[:, fs, a:b], s_sb[si][:], rstd_c_bc[:, a:b], ALU.mult, ["s_sb%d" % si, "rstd_c_bc"], [hk])
                    for dk in range(DC):
                        for ti, (a, b) in enumerate(TCH):
                            py = 4 + yrot[0] % 3
                            yrot[0] += 1
                            for fs in range(2):
                                MM(ps[py][:, 0:352], dslot[di][:, fs, dk * 128:(dk + 1) * 128], hd[:, fs, a:b], fs == 0, fs == 1, [hk, dk_], ["ps%d" % py])
                            if a == 0:
                                STT("dve", xT[:, dk, 0:CTXL], ps[py][:, 0:CTXL], modS[:, 5, dk, 1:2], xT[:, dk, 0:CTXL], ALU.mult, ALU.add,
                                    ["ps%d" % py, "modS", "x%d" % dk], ["x%d" % dk])
                                STT("dve", xT[:, dk, CTXL:b], ps[py][:, CTXL:352], modS[:, 5, dk, 0:1], xT[:, dk, CTXL:b], ALU.mult, ALU.add,
                                    ["ps%d" % py, "modS", "x%d" % dk], ["x%d" % dk])
                            else:
                                STT("dve", xT[:, dk, a:b], ps[py][:, 0:352], modS[:, 5, dk, 0:1], xT[:, dk, a:b], ALU.mult, ALU.add,
                                    ["ps%d" % py, "modS", "x%d" % dk], ["x%d" % dk])
                fence(BFK)

        for k in range(DC):
            P.dma("sp", out_d[k * 128:(k + 1) * 128, :], xT[:, k, CTXL:T], reads=["x%d" % k])
        if debug == 1 and n_exp > 0:
            for k in range(DC):
                P.dma("sp", dbg_d[k * 128:(k + 1) * 128, :], xT[:, k, :], reads=["x%d" % k])
        P.emit()
    return nc


def make_in_maps(inp, L, n_exp=NEXP):
    f = np.float32
    x = np.asarray(inp["x"], f)[0]
    ctx = np.asarray(inp["ctx"], f)[0]
    c = np.asarray(inp["c"], f)[0]
    c_ctx = np.asarray(inp["c_ctx"], f)
    ccT = np.stack([c.reshape(DC, 128).T, c_ctx.reshape(DC, 128).T], axis=-1).astype(f)
    badaT = np.ascontiguousarray(np.asarray(inp["b_ada"], f)[:L].reshape(L, 96, 128).transpose(0, 2, 1))
    g_attn = np.asarray(inp["g_attn"], f)[:L].reshape(L, DC, 128).transpose(0, 2, 1)
    g_ffn = np.asarray(inp["g_ffn"], f)[:L].reshape(L, DC, 128).transpose(0, 2, 1)
    g_oa = np.asarray(inp["g_out_attn"], f)[:L].reshape(L, 8, 128).transpose(0, 2, 1)
    g_oc = np.asarray(inp["g_out_conv"], f)[:L].reshape(L, 8, 128).transpose(0, 2, 1)
    cw = np.asarray(inp["conv_w"], f)[:L].reshape(L, 3, 8, 128).transpose(0, 3, 2, 1).reshape(L, 128, 24)
    pvec = np.ascontiguousarray(np.concatenate([g_attn, g_ffn, g_oa, g_oc, cw], axis=2))
    qkg = np.concatenate([np.asarray(inp["q_norm_g"], f)[:L], np.asarray(inp["k_norm_g"], f)[:L]], axis=1)
    qkg = np.ascontiguousarray(np.broadcast_to(qkg[:, None, :], (L, 128, 128)))
    sinkb = np.ascontiguousarray(np.broadcast_to(np.asarray(inp["sink"], f)[:L][:, None, :], (L, 128, 16)))
    rbias = np.ascontiguousarray(np.broadcast_to(np.asarray(inp["router_bias"], f)[None, :], (128, 16)))
    wrT = np.ascontiguousarray(np.asarray(inp["w_router"], f).reshape(DC, 128, 16).transpose(1, 0, 2))
    inv_freq = (10000.0 ** (-np.arange(0, 32, 2, dtype=f) / f(32))).astype(f)
    masks = np.zeros((128, 2, 128), f)
    jj = np.arange(128)[:, None]
    ii = np.arange(128)[None, :]
    masks[:, 0, :] = (jj >= ii)
    masks[:, 1, :] = (jj <= ii)
    shared = dict(ccT=ccT, w_ada=np.asarray(inp["w_ada"], f)[:L], badaT=badaT, w_in=np.asarray(inp["w_in"], f)[:L],
                  w_out=np.asarray(inp["w_out"], f)[:L], w_exp_gate=np.asarray(inp["w_exp_gate"], f)[:L, :max(n_exp, 1)],
                  w_exp_up=np.asarray(inp["w_exp_up"], f)[:L, :max(n_exp, 1)], w_exp_down=np.asarray(inp["w_exp_down"], f)[:L, :max(n_exp, 1)],
                  pvec=pvec, qkg=qkg, sinkb=sinkb, rbias=rbias, w_routerT=wrT, masks=masks)
    maps = []
    for cid in range(NCORES):
        xs = np.concatenate([ctx[CTXL * cid:CTXL * (cid + 1)], x[LAT * cid:LAT * (cid + 1)]], axis=0)
        xT = np.ascontiguousarray(xs.T)
        pos = np.arange(LAT * cid, LAT * (cid + 1))
        row_id = (pos // 64).astype(f)
        col_id = (pos % 64).astype(f)
        ar = row_id[:, None] * inv_freq[None, :]
        ac = col_id[:, None] * inv_freq[None, :]
        cr, sr, cc_, sc = np.cos(ar).astype(f), np.sin(ar).astype(f), np.cos(ac).astype(f), np.sin(ac).astype(f)
        cos64 = np.concatenate([cr, cr, cc_, cc_], axis=1)
        sin64 = np.concatenate([-sr, sr, -sc, sc], axis=1)
        ropec = np.ones((128, NT, 64), f)
        ropes = np.zeros((128, NT, 64), f)
        ropec[:, 1:, :] = cos64.reshape(8, 128, 64).transpose(1, 0, 2)
        ropes[:, 1:, :] = sin64.reshape(8, 128, 64).transpose(1, 0, 2)
        selkv = np.zeros((128, 16), f)
        if cid > 0:
            selkv[:, cid - 1] = 1.0
        if cid < NCORES - 1:
            selkv[:, 8 + cid + 1] = 1.0
        selcu = np.zeros((32, 4), f)
        if cid > 0:
            selcu[4 * (cid - 1) + 1, 0] = 1.0
            selcu[4 * (cid - 1) + 3, 2] = 1.0
        if cid < NCORES - 1:
            selcu[4 * (cid + 1) + 0, 1] = 1.0
            selcu[4 * (cid + 1) + 2, 3] = 1.0
        m = dict(shared)
        m.update(xT=xT, ropec=ropec, ropes=ropes, selkv=selkv, selcu=selcu)
        maps.append(m)
    return maps


_NC_CACHE = {}


def run(inp, L, n_exp=NEXP, debug=False):
    key = (L, n_exp, debug)
    if key not in _NC_CACHE:
        _NC_CACHE[key] = build_program(L, n_exp, debug)
    nc = _NC_CACHE[key]
    maps = make_in_maps(inp, L, n_exp)
    res = run_bass_kernel_spmd(nc, maps, core_ids=list(range(NCORES)))
    out = np.concatenate([np.ascontiguousarray(r["outT"].T) for r in res.results], axis=0)[None]
    if debug:
        return out.astype(np.float32), [r["dbg"] for r in res.results]
    return out.astype(np.float32)


def kernel(**inputs):
    return run(inputs, 4)
```
